# Optimizing a Trainium2 kernel written in Bass

```python
import jax, jax.numpy as jnp
from jax import lax
import numpy as np

D_MODEL = 2048
BATCH = 4
SEQ = 2048
DEPTH = 1

GRID_W = 64
CTX_LEN = 256
M_HEADS = 8
QK_DIM = 128
V_DIM = 256
QK_WIDTH = M_HEADS * QK_DIM
V_WIDTH = M_HEADS * V_DIM
CONV_W = 5
MLSTM_CHUNK = 128
SGU_GROUPS = 8
SGU_CHUNK = 128
SGU_WIDTH = 2048
SGU_GDIM = SGU_WIDTH // SGU_GROUPS
ROWS_PER_CHUNK = SGU_CHUNK // GRID_W
N_EXPERTS = 32
TOP_K = 4
D_EXPERT = 2048
SWIGLU_ALPHA = 1.702
SWIGLU_LIMIT = 7.0
MOE_BLOCK = 256
EPS = 1e-6
N_MOD = 6

OFF_Q = 0
OFF_K = OFF_Q + QK_WIDTH
OFF_V = OFF_K + QK_WIDTH
OFF_GATES = OFF_V + V_WIDTH
OFF_O = OFF_GATES + 4 * M_HEADS
OFF_U = OFF_O + V_WIDTH
OFF_SV = OFF_U + SGU_WIDTH
OFF_MERGE = OFF_SV + SGU_WIDTH
IN_COLS = OFF_MERGE + 2 * D_MODEL

kernel_name = 'hybrid_mlstm_chunkmlp_moe_dit_block'


def rmsnorm(x, g):
    xf = x.astype(jnp.float32)
    y = xf * lax.rsqrt(jnp.mean(xf * xf, axis=-1, keepdims=True) + EPS)
    return (y * g).astype(x.dtype)


def modulate(h, shift, scale):
    return h * (1 + scale) + shift


def heads(a, d):
    B, T, _ = a.shape
    return a.reshape(B, T, M_HEADS, d).transpose(0, 2, 1, 3)


def centred_dwconv(x, w, b):
    C = x.shape[-1]
    y = lax.conv_general_dilated(x, w[:, None, :].astype(x.dtype), (1,), [(CONV_W // 2, CONV_W // 2)],
                                 dimension_numbers=('NWC', 'WIO', 'NWC'), feature_group_count=C)
    return y + b


def in_projection(h, w_in, gate_b, conv_w, conv_b, full):
    B, T, _ = h.shape
    z = h @ (w_in if full else w_in[:, :OFF_O])
    qk = jax.nn.silu(centred_dwconv(z[..., :OFF_V], conv_w, conv_b))
    q = heads(qk[..., :QK_WIDTH], QK_DIM) * (QK_DIM ** -0.5)
    k = heads(qk[..., QK_WIDTH:], QK_DIM)
    v = heads(z[..., OFF_V:OFF_GATES], V_DIM)
    gates = z[..., OFF_GATES:OFF_O].astype(jnp.float32).reshape(B, T, 4, M_HEADS) + gate_b
    gates = gates.transpose(0, 2, 3, 1)
    mix = (q, k, v, gates)
    if not full:
        return mix, None
    rest = (z[..., OFF_O:OFF_U], z[..., OFF_U:OFF_SV], z[..., OFF_SV:OFF_MERGE], z[..., OFF_MERGE:])
    return mix, rest


def to_chunks(a):
    B, H, T = a.shape[:3]
    a = a.reshape(B, H, T // MLSTM_CHUNK, MLSTM_CHUNK, *a.shape[3:])
    return jnp.moveaxis(a, 2, 0)


def mlstm_chunked(q, k, v, i_pre, f_pre):
    B, H, T, _ = q.shape
    L = MLSTM_CHUNK
    tri = jnp.tril(jnp.ones((L, L), dtype=bool))
    log_f = jax.nn.log_sigmoid(f_pre)

    def step(carry, blk):
        C, n, m = carry
        qb, kb, vb, ib, lfb = blk
        b = jnp.cumsum(lfb, axis=-1)
        d_log = jnp.where(tri, b[..., :, None] - b[..., None, :] + ib[..., None, :], -jnp.inf)
        inter = b + m[..., None]
        m_t = jnp.maximum(inter, jnp.max(d_log, axis=-1))
        w_inter = jnp.exp(inter - m_t)
        s = jnp.einsum('bhtd,bhsd->bhts', qb, kb).astype(jnp.float32) * jnp.exp(d_log - m_t[..., None])
        num = w_inter[..., None] * jnp.einsum('bhtd,bhde->bhte', qb, C) + jnp.einsum('bhts,bhse->bhte', s, vb)
        den = w_inter * jnp.einsum('bhtd,bhd->bht', qb, n) + jnp.sum(s, axis=-1)
        h = num / jnp.maximum(jnp.abs(den), jnp.exp(-m_t))[..., None]
        b_end = b[..., -1]
        log_w = b_end[..., None] - b + ib
        m_new = jnp.maximum(b_end + m, jnp.max(log_w, axis=-1))
        decay = jnp.exp(b_end + m - m_new)
        w = jnp.exp(log_w - m_new[..., None])
        C = decay[..., None, None] * C + jnp.einsum('bhs,bhsd,bhse->bhde', w, kb, vb)
        n = decay[..., None] * n + jnp.einsum('bhs,bhsd->bhd', w, kb)
        return (C, n, m_new), h

    state0 = (jnp.zeros((B, H, QK_DIM, V_DIM), jnp.float32),
              jnp.zeros((B, H, QK_DIM), jnp.float32),
              jnp.zeros((B, H), jnp.float32))
    blocks = (to_chunks(q), to_chunks(k), to_chunks(v), to_chunks(i_pre), to_chunks(log_f))
    _, hs = lax.scan(step, state0, blocks)
    return jnp.moveaxis(hs, 0, 2).reshape(B, H, T, V_DIM)


def mlstm_bidirectional(mix_ctx, mix_lat):
    qc, kc, vc, gc = mix_ctx
    ql, kl, vl, gl = mix_lat
    Tc = qc.shape[2]
    cat = lambda a, b: jnp.concatenate([a, b], axis=2)
    rev = lambda a: jnp.flip(a, axis=2)
    h_f = mlstm_chunked(cat(qc, ql), cat(kc, kl), cat(vc, vl), cat(gc[:, 0], gl[:, 0]), cat(gc[:, 1], gl[:, 1]))
    h_b = mlstm_chunked(cat(rev(qc), rev(ql)), cat(rev(kc), rev(kl)), cat(rev(vc), rev(vl)),
                        cat(rev(gc[:, 2]), rev(gl[:, 2])), cat(rev(gc[:, 3]), rev(gl[:, 3])))
    h_lat = h_f[:, :, Tc:] + rev(h_b[:, :, Tc:])
    h_ctx = h_f[:, :, :Tc] + rev(h_b[:, :, :Tc])
    return h_lat, h_ctx


def mlstm_readout(h, o, g):
    B, H, T, _ = h.shape
    h = rmsnorm(h.transpose(0, 2, 1, 3), g.reshape(H, V_DIM))
    return h.reshape(B, T, V_WIDTH) * jax.nn.sigmoid(o)


def chunk_mlp(u, sv, n_chunks, g, sgu_w, sgu_b):
    B, T, _ = u.shape
    u = jax.nn.gelu(u)
    sv = rmsnorm(jax.nn.gelu(sv), g)
    vc = sv.reshape(B, n_chunks, SGU_CHUNK, SGU_GROUPS, SGU_GDIM)
    z = jnp.einsum('gts,bnsgd->bntgd', sgu_w, vc) + sgu_b.T[None, None, :, :, None]
    return u * z.reshape(B, T, SGU_WIDTH)


def token_mixer(h_lat, h_ctx, n_lat_chunks, with_ctx_out, w_in, gate_b, conv_w, conv_b, mlstm_norm_g,
                sgu_norm_g, sgu_w, sgu_b, proj_a, proj_b, w_out):
    mix_lat, rest_lat = in_projection(h_lat, w_in, gate_b, conv_w, conv_b, True)
    mix_ctx, rest_ctx = in_projection(h_ctx, w_in, gate_b, conv_w, conv_b, with_ctx_out)
    hm_lat, hm_ctx = mlstm_bidirectional(mix_ctx, mix_lat)

    def merge(hm, rest, n_chunks, dtype):
        o, u, sv, mg = rest
        y_a = mlstm_readout(hm, o, mlstm_norm_g).astype(dtype) @ proj_a
        y_b = chunk_mlp(u, sv, n_chunks, sgu_norm_g, sgu_w, sgu_b) @ proj_b
        g_a = jax.nn.sigmoid(mg[..., :D_MODEL])
        g_b = jax.nn.sigmoid(mg[..., D_MODEL:])
        return (g_a * y_a + g_b * y_b) @ w_out

    out_lat = merge(hm_lat, rest_lat, n_lat_chunks, h_lat.dtype)
    out_ctx = merge(hm_ctx, rest_ctx, h_ctx.shape[1] // SGU_CHUNK, h_ctx.dtype) if with_ctx_out else None
    return out_lat, out_ctx


def clamped_swiglu(hb):
    glu = jnp.minimum(hb[..., :D_EXPERT], SWIGLU_LIMIT)
    lin = jnp.clip(hb[..., D_EXPERT:], -SWIGLU_LIMIT, SWIGLU_LIMIT)
    return glu * jax.nn.sigmoid(SWIGLU_ALPHA * glu) * (lin + 1)


def moe_ffn(xt, router_w, router_b, w1, b1, w2, b2):
    N = xt.shape[0]
    logits = (xt @ router_w).astype(jnp.float32) + router_b
    top_val, top_idx = lax.top_k(logits, TOP_K)
    gates = jax.nn.softmax(top_val, axis=-1)
    M = N * TOP_K
    eid = top_idx.reshape(M)
    tok = jnp.arange(M, dtype=jnp.int32) // TOP_K
    gate = gates.reshape(M)
    order = jnp.argsort(eid)
    e_sorted = eid[order]
    counts = jnp.zeros((N_EXPERTS,), jnp.int32).at[eid].add(1)
    padded = (counts + MOE_BLOCK - 1) // MOE_BLOCK * MOE_BLOCK
    start_sorted = jnp.cumsum(counts) - counts
    pad_end = jnp.cumsum(padded)
    start_pad = pad_end - padded
    dest = start_pad[e_sorted] + jnp.arange(M, dtype=jnp.int32) - start_sorted[e_sorted]
    n_blocks = -(-M // MOE_BLOCK) + N_EXPERTS
    P = n_blocks * MOE_BLOCK
    buf_tok = jnp.zeros((P,), jnp.int32).at[dest].set(tok[order])
    buf_gate = jnp.zeros((P,), jnp.float32).at[dest].set(gate[order])
    block_start = jnp.arange(n_blocks, dtype=jnp.int32) * MOE_BLOCK
    block_e = jnp.minimum(jnp.searchsorted(pad_end, block_start, side='right'), N_EXPERTS - 1)

    def body(acc, blk):
        tok_b, gate_b, e = blk
        hb = xt[tok_b] @ w1[e] + b1[e]
        y = clamped_swiglu(hb) @ w2[e] + b2[e]
        return acc.at[tok_b].add(gate_b[:, None] * y.astype(jnp.float32)), None

    acc, _ = lax.scan(body, jnp.zeros(xt.shape, jnp.float32),
                      (buf_tok.reshape(n_blocks, MOE_BLOCK), buf_gate.reshape(n_blocks, MOE_BLOCK), block_e))
    return acc.astype(xt.dtype)


def setup_inputs(seed: int = 0) -> dict:
    key = jax.random.key(seed)
    ks = jax.random.split(key, 28)
    nrm = lambda k, shape, s: jax.random.normal(k, shape, jnp.float32) * s
    f_bias = jnp.linspace(3.0, 6.0, M_HEADS)
    zh = jnp.zeros((M_HEADS,), jnp.float32)
    gate_base = jnp.stack([zh, f_bias, zh, f_bias])
    return {
        'x': nrm(ks[0], (BATCH, SEQ, D_MODEL), 1.0),
        'c': nrm(ks[1], (BATCH, D_MODEL), 1.0),
        'ctx': nrm(ks[2], (BATCH, CTX_LEN, D_MODEL), 1.0),
        'c_ctx': nrm(ks[3], (D_MODEL,), 1.0),
        'mod_w': nrm(ks[4], (DEPTH, D_MODEL, N_MOD * D_MODEL), 0.5 * D_MODEL ** -0.5),
        'mod_b': nrm(ks[5], (DEPTH, N_MOD * D_MODEL), 0.02),
        'norm1_g': 1.0 + nrm(ks[6], (DEPTH, D_MODEL), 0.1),
        'norm2_g': 1.0 + nrm(ks[7], (DEPTH, D_MODEL), 0.1),
        'w_in': nrm(ks[8], (DEPTH, D_MODEL, IN_COLS), D_MODEL ** -0.5),
        'gate_b': gate_base[None] + nrm(ks[9], (DEPTH, 4, M_HEADS), 0.1),
        'conv_w': nrm(ks[10], (DEPTH, CONV_W, 2 * QK_WIDTH), CONV_W ** -0.5),
        'conv_b': nrm(ks[11], (DEPTH, 2 * QK_WIDTH), 0.02),
        'mlstm_norm_g': 1.0 + nrm(ks[12], (DEPTH, V_WIDTH), 0.1),
        'sgu_norm_g': 1.0 + nrm(ks[13], (DEPTH, SGU_WIDTH), 0.1),
        'sgu_w': nrm(ks[14], (DEPTH, SGU_GROUPS, SGU_CHUNK, SGU_CHUNK), SGU_CHUNK ** -0.5),
        'sgu_b': 1.0 + nrm(ks[15], (DEPTH, SGU_GROUPS, SGU_CHUNK), 0.1),
        'proj_a': nrm(ks[16], (DEPTH, V_WIDTH, D_MODEL), V_WIDTH ** -0.5),
        'proj_b': nrm(ks[17], (DEPTH, SGU_WIDTH, D_MODEL), SGU_WIDTH ** -0.5),
        'w_out': nrm(ks[18], (DEPTH, D_MODEL, D_MODEL), D_MODEL ** -0.5),
        'router_w': nrm(ks[19], (DEPTH, D_MODEL, N_EXPERTS), D_MODEL ** -0.5),
        'router_b': nrm(ks[20], (DEPTH, N_EXPERTS), 0.01),
        'exp_w1': nrm(ks[21], (DEPTH, N_EXPERTS, D_MODEL, 2 * D_EXPERT), D_MODEL ** -0.5),
        'exp_b1': nrm(ks[22], (DEPTH, N_EXPERTS, 2 * D_EXPERT), 0.01),
        'exp_w2': nrm(ks[23], (DEPTH, N_EXPERTS, D_EXPERT, D_MODEL), D_EXPERT ** -0.5),
        'exp_b2': nrm(ks[24], (DEPTH, N_EXPERTS, D_MODEL), 0.01),
        'final_g': 1.0 + nrm(ks[25], (D_MODEL,), 0.1),
    }


def reference(x, c, ctx, c_ctx, mod_w, mod_b, norm1_g, norm2_g, w_in, gate_b, conv_w, conv_b,
              mlstm_norm_g, sgu_norm_g, sgu_w, sgu_b, proj_a, proj_b, w_out, router_w, router_b,
              exp_w1, exp_b1, exp_w2, exp_b2, final_g):
    B, T, _ = x.shape
    rows = T // GRID_W
    n_lat_chunks = rows // ROWS_PER_CHUNK
    for l in range(DEPTH):
        last = l == DEPTH - 1
        mod_lat = (jax.nn.silu(c) @ mod_w[l] + mod_b[l]).reshape(B, 1, N_MOD, D_MODEL)
        mod_ctx = (jax.nn.silu(c_ctx) @ mod_w[l] + mod_b[l]).reshape(1, 1, N_MOD, D_MODEL)
        h_lat = modulate(rmsnorm(x, norm1_g[l]), mod_lat[:, :, 0], mod_lat[:, :, 1])
        h_ctx = modulate(rmsnorm(ctx, norm1_g[l]), mod_ctx[:, :, 0], mod_ctx[:, :, 1])
        out_lat, out_ctx = token_mixer(h_lat, h_ctx, n_lat_chunks, not last, w_in[l], gate_b[l], conv_w[l],
                                       conv_b[l], mlstm_norm_g[l], sgu_norm_g[l], sgu_w[l], sgu_b[l],
                                       proj_a[l], proj_b[l], w_out[l])
        x = x + mod_lat[:, :, 2] * out_lat
        h2 = modulate(rmsnorm(x, norm2_g[l]), mod_lat[:, :, 3], mod_lat[:, :, 4])
        y2 = moe_ffn(h2.reshape(B * T, D_MODEL), router_w[l], router_b[l], exp_w1[l], exp_b1[l],
                     exp_w2[l], exp_b2[l]).reshape(B, T, D_MODEL)
        x = x + mod_lat[:, :, 5] * y2
        if not last:
            Tc = ctx.shape[1]
            ctx = ctx + mod_ctx[:, :, 2] * out_ctx
            h2c = modulate(rmsnorm(ctx, norm2_g[l]), mod_ctx[:, :, 3], mod_ctx[:, :, 4])
            y2c = moe_ffn(h2c.reshape(B * Tc, D_MODEL), router_w[l], router_b[l], exp_w1[l], exp_b1[l],
                          exp_w2[l], exp_b2[l]).reshape(B, Tc, D_MODEL)
            ctx = ctx + mod_ctx[:, :, 5] * y2c
    return rmsnorm(x, final_g)
```

```python
import numpy as np
from contextlib import ExitStack
import concourse.bass as bass
import concourse.mybir as mybir
from concourse.bass_utils import run_bass_kernel_spmd

F32 = mybir.dt.float32
BF16 = mybir.dt.bfloat16
AF = mybir.ActivationFunctionType
ALU = mybir.AluOpType
AX = mybir.AxisListType

D = 2048
NCH = 16
TOWN = 1024
TCTX = 256
H = 8
NE = 32
OFF_Q, OFF_K, OFF_V, OFF_G, OFF_O = 0, 1024, 2048, 4096, 4128
OFF_U = OFF_O + 2048
OFF_SV = OFF_U + 2048
OFF_MG = OFF_SV + 2048
EPS = 1e-6
DBG = None
STOP_AFTER = None
N_EXPERTS_RUN = None
P3_HEADS = 8
P3_PART = 9
P3_STEPS = 99
P3_SKIPK = 0
P3_SKIPV = 0
NO_CC = 0

ENGS = ("pe", "act", "dve", "pool", "sp")
NDMASEM = 6


class Ev:
    __slots__ = ("eng", "kind", "sem", "val", "needed")

    def __init__(self, eng, kind):
        self.eng = eng
        self.kind = kind
        self.sem = None
        self.val = None
        self.needed = False


class _St:
    __slots__ = ("w", "r")

    def __init__(self):
        self.w = None
        self.r = []


class Res:
    def __init__(self, name, excl=False):
        self.name = name
        self.excl = excl
        self.whole = _St()
        self.subs = {}

    def __getitem__(self, key):
        return (self, key)


class _Op:
    __slots__ = ("fn", "deps", "ev", "guard")

    def __init__(self, fn, deps, ev, guard):
        self.fn = fn
        self.deps = deps
        self.ev = ev
        self.guard = guard


class Prog:
    def __init__(self, nc, stack):
        self.nc = nc
        self.ops = {e: [] for e in ENGS}
        self.esem = {e: stack.enter_context(nc.semaphore("es_" + e)) for e in ENGS}
        self.dsem = {}
        self.duse = {}
        self.drr = {}
        for q in ("sp", "act", "pool"):
            self.dsem[q] = [stack.enter_context(nc.semaphore("ds_%s%d" % (q, i))) for i in range(NDMASEM)]
            self.duse[q] = [0] * NDMASEM
            self.drr[q] = 0
        self.ccsem = [stack.enter_context(nc.semaphore("cc%d" % i)) for i in range(8)]
        self.ccn = 0
        self.out_evs = []
        self.live_dma = []
        self.last = {e: None for e in ENGS}
        self.bar_pending = {e: [] for e in ENGS}

    @staticmethod
    def _norm(r):
        if isinstance(r, Res):
            return r, None
        return r

    def _collect(self, eng, reads, writes, is_dma=False):
        deps = []

        def add(ev, raw):
            if ev is None:
                return
            if ev.eng == eng and ev.kind == "eng" and not is_dma:
                if eng == "pe" or not raw:
                    return
            deps.append(ev)

        for r in reads:
            res, key = self._norm(r)
            add(res.whole.w, True)
            if key is None:
                for st in res.subs.values():
                    add(st.w, True)
            else:
                st = res.subs.get(key)
                if st is not None:
                    add(st.w, True)
        for r in writes:
            res, key = self._norm(r)
            sts = [res.whole]
            if key is None:
                sts += list(res.subs.values())
            else:
                st = res.subs.get(key)
                if st is not None:
                    sts.append(st)
            for st in sts:
                add(st.w, False)
                for ev in st.r:
                    add(ev, False)
        if self.bar_pending[eng]:
            deps += self.bar_pending[eng]
            self.bar_pending[eng] = []
        return deps

    def _commit(self, ev, reads, writes):
        for r in reads:
            res, key = self._norm(r)
            if key is None:
                res.whole.r.append(ev)
            else:
                res.subs.setdefault(key, _St()).r.append(ev)
        for r in writes:
            res, key = self._norm(r)
            if key is None:
                res.whole.w = ev
                res.whole.r = []
                res.subs = {}
            else:
                st = res.subs.setdefault(key, _St())
                st.w = ev
                st.r = []

    def op(self, eng, fn, reads=(), writes=()):
        ex = [r for r in reads if self._norm(r)[0].excl]
        if ex:
            reads = [r for r in reads if not self._norm(r)[0].excl]
            writes = list(writes) + ex
        deps = self._collect(eng, reads, writes)
        for d in deps:
            d.needed = True
        ev = Ev(eng, "eng")
        self.ops[eng].append(_Op(fn, deps, ev, None))
        self._commit(ev, reads, writes)
        self.last[eng] = ev
        return ev

    def dma(self, q, out, in_, reads=(), writes=(), is_output=False, **kw):
        deps = self._collect(q, reads, writes, True)
        for d in deps:
            d.needed = True
        i = self.drr[q]
        self.drr[q] = (i + 1) % NDMASEM
        ev = Ev(q, "dma")
        ev.sem = self.dsem[q][i]
        guard = (ev.sem, 16 * self.duse[q][i]) if self.duse[q][i] > 0 else None
        self.duse[q][i] += 1
        ev.val = 16 * self.duse[q][i]
        ev.needed = True

        def fn(h, out=out, in_=in_, kw=kw):
            return h.dma_start(out=out, in_=in_, **kw)

        self.ops[q].append(_Op(fn, deps, ev, guard))
        self._commit(ev, reads, writes)
        self.live_dma.append(ev)
        if is_output:
            self.out_evs.append(ev)
        return ev

    def dmaop(self, q, fn, reads=(), writes=()):
        deps = self._collect(q, reads, writes, True)
        for d in deps:
            d.needed = True
        i = self.drr[q]
        self.drr[q] = (i + 1) % NDMASEM
        ev = Ev(q, "dma")
        ev.sem = self.dsem[q][i]
        guard = (ev.sem, 16 * self.duse[q][i]) if self.duse[q][i] > 0 else None
        self.duse[q][i] += 1
        ev.val = 16 * self.duse[q][i]
        ev.needed = True
        self.ops[q].append(_Op(fn, deps, ev, guard))
        self._commit(ev, reads, writes)
        self.live_dma.append(ev)
        return ev

    def ccop(self, q, fn, reads=(), writes=()):
        deps = self._collect(q, reads, writes, True)
        for d in deps:
            d.needed = True
        ev = Ev(q, "cc")
        ev.sem = self.ccsem[self.ccn]
        self.ccn += 1
        ev.val = 1
        ev.needed = True
        self.ops[q].append(_Op(fn, deps, ev, None))
        self._commit(ev, reads, writes)
        self.live_dma.append(ev)
        return ev

    def barrier(self, keep=()):
        kept = [e for e in self.live_dma if e in keep]
        evs = [e for e in self.last.values() if e is not None] + [e for e in self.live_dma if e not in keep]
        for e in evs:
            e.needed = True
        self.live_dma = kept
        for e in ENGS:
            self.bar_pending[e] = list(evs)

    def emit(self, final_eng="sp"):
        for e in ENGS:
            c = 0
            for o in self.ops[e]:
                if o.ev.kind == "eng":
                    o.ev.sem = self.esem[e]
                    if o.ev.needed:
                        c += 1
                        o.ev.val = c
        nc = self.nc
        with nc.Block() as block:
            for e in ENGS:
                ops = self.ops[e]
                extra = self.out_evs if e == final_eng else []
                if not ops and not extra:
                    continue

                def body(h, ops=ops, extra=extra, me=e):
                    waited = {}

                    def wait(sem, val):
                        k = id(sem)
                        if waited.get(k, 0) >= val:
                            return
                        waited[k] = val
                        h.wait_ge(sem, val)

                    for o in ops:
                        if o.guard is not None:
                            wait(*o.guard)
                        for d in o.deps:
                            if d is o.ev:
                                continue
                            if d.eng == me and d.kind == "eng" and me == "pe":
                                continue
                            wait(d.sem, d.val)
                        ins = o.fn(h)
                        if o.ev.kind == "dma":
                            ins.then_inc(o.ev.sem, 16)
                        elif o.ev.kind == "cc":
                            ins.then_inc(o.ev.sem, 1)
                        elif o.ev.needed:
                            ins.then_inc(o.ev.sem, 1)
                    for d in extra:
                        wait(d.sem, d.val)

                getattr(block, {"pe": "tensor", "act": "scalar", "dve": "vector",
                                "pool": "gpsimd", "sp": "sync"}[e])(body)


KB = 1024
ARENA_KB = 206


class Arena:
    def __init__(self, nc, st):
        self.t = st.enter_context(nc.sbuf_tensor("arena", [128, ARENA_KB * KB // 2], BF16))

    def view(self, off, shape, dt, parts=128):
        n = 1
        for s in shape:
            n *= s
        esz = 4 if dt == F32 else 2
        assert off % 4 == 0 and off + n * esz <= ARENA_KB * KB, (off, shape)
        a = off // 2
        ap = self.t[0:parts, a:a + n * esz // 2]
        if dt == F32:
            ap = ap.bitcast(F32)
        if len(shape) > 1:
            names = "abcde"[:len(shape)]
            pat = "p (%s) -> p %s" % (" ".join(names), " ".join(names))
            ap = ap.rearrange(pat, **{names[i]: shape[i] for i in range(len(shape))})
        return ap


class Bump:
    def __init__(self, arena, lo, hi):
        self.arena, self.cur, self.hi = arena, lo, hi

    def __call__(self, shape, dt=F32, parts=128):
        n = 1
        for s in shape:
            n *= s
        nb = n * (4 if dt == F32 else 2)
        nb = (nb + 63) // 64 * 64
        off = self.cur
        self.cur += nb
        assert self.cur <= self.hi, ("SBUF region overflow", shape, self.cur, self.hi)
        return self.arena.view(off, shape, dt, parts)


def build_program():
    nc = bass.Bass("TRN2", target_bir_lowering=False)

    T = {}
    for name, shape in [
        ("xT_own", (D, TOWN)), ("xT_oth", (D, TOWN)), ("xT_ctx", (D, TCTX)), ("x_own", (TOWN, D)),
        ("cfm", (128, 32)), ("mod_w", (D, 6 * D)), ("modb_fm", (128, 96)), ("modb_row", (1, 6 * D)),
        ("n1g_fm", (128, 16)), ("n2g_bc", (128, D)), ("fing_bc", (128, D)),
        ("w_in", (D, 14368)), ("wg", (D, 32)), ("gateb_bc", (128, 32)),
        ("convw_fm", (128, 80)), ("convb_fm", (128, 16)),
        ("mng_bc", (128, D)), ("sng_bc", (128, D)), ("sgwT", (128, 1024)), ("sgb_row", (1, 1024)),
        ("proj_a", (D, D)), ("proj_b", (D, D)), ("w_out", (D, D)),
        ("router_w", (D, 32)), ("routerb_bc", (128, 32)),
        ("exp_w1", (4, D, 2 * D)), ("exp_w2", (4, D, D)), ("b1_fm", (128, NE * 32)), ("exp_b2", (NE, D)),
        ("ident", (128, 128)), ("maskf", (128, 128)), ("maskb", (128, 128)),
    ]:
        if name in ("exp_w1", "exp_w2") and STOP_AFTER is not None and STOP_AFTER < 7:
            continue
        T[name] = nc.dram_tensor(name, list(shape), F32, kind="ExternalInput")
    out_d = nc.dram_tensor("out", [TOWN, D], F32, kind="ExternalOutput")
    modrow_d = nc.dram_tensor("modrow_scr", [1, 6 * D], F32)
    x1_d = nc.dram_tensor("x1_scr", [TOWN, D], F32)
    if STOP_AFTER is None or STOP_AFTER >= 7:
        SH1 = [nc.dram_tensor("sh1_%d" % k, [8 * 128, 8192], BF16) for k in range(4)]
        SH2 = [nc.dram_tensor("sh2_%d" % k, [4 * 128, 8192], BF16) for k in range(4)]
        G1 = [nc.dram_tensor("g1_%d" % k, [8 * 8 * 128, 8192], BF16) for k in range(4)]
        G2 = [nc.dram_tensor("g2_%d" % k, [8 * 4 * 128, 8192], BF16) for k in range(4)]
    dbg_out = {}
    stop = 99 if STOP_AFTER is None else STOP_AFTER

    with ExitStack() as st:
        P = Prog(nc, st)
        AR = Arena(nc, st)
        PS = [st.enter_context(nc.psum_tensor("ps%d" % i, [128, 512], F32))[:, :] for i in range(8)]
        RPS = [Res("ps%d" % i, excl=True) for i in range(8)]

        def dbg(name, ap, shape, reads, dt=F32):
            if DBG is None or name not in DBG:
                return
            t = nc.dram_tensor("dbg_" + name, list(shape), dt, kind="ExternalOutput")
            dbg_out[name] = t
            P.dma("sp", t.ap(), ap, reads=reads, is_output=True)

        S0 = Bump(AR, 0, 8 * KB)
        ident_f = S0([128]); ident_b = S0([128], BF16)
        maskf = S0([128]); maskb = S0([128])
        ones_b = S0([128], BF16); ones_f = S0([128])
        modfm = S0([64]); A1 = S0([32]); B1 = S0([32])
        GT = S0([8, 32])
        sgb = S0([8, 128], BF16, parts=1)
        RC = Res("consts"); Rmod = Res("modfm"); RGT = Res("GT"); Rsgb = Res("sgb")
        masks = [maskf, maskb]
        P.dma("sp", ident_f, T["ident"].ap(), writes=[RC["i"]])
        P.dma("sp", maskf, T["maskf"].ap(), writes=[RC["mf"]])
        P.dma("sp", maskb, T["maskb"].ap(), writes=[RC["mb"]])
        P.op("dve", lambda h: h.tensor_copy(ident_b, ident_f), reads=[RC["i"]], writes=[RC["ib"]])
        P.op("dve", lambda h: h.memset(ones_b, 1.0), writes=[RC["ob"]])
        P.op("dve", lambda h: h.memset(ones_f, 1.0), writes=[RC["of"]])

        BA_OFF, BB_OFF, BC_OFF = 8 * KB, 40 * KB, 72 * KB
        hT_own = AR.view(BA_OFF, [16, TOWN], BF16); RBA = Res("BA")
        ROT = AR.view(BB_OFF, [16, TOWN], BF16); RBB = Res("BB")
        MA = AR.view(BC_OFF, [16, TOWN], BF16); RBC = Res("BC")
        RhT_own = RBA

        def gelu_tile(src_ps, Rsrc, dst_ap, Rdst, tmpA, tmpB, RtA, RtB):
            P.op("dve", lambda h: h.tensor_copy(tmpB, src_ps), reads=[Rsrc], writes=[RtB])
            P.op("act", lambda h: h.activation(tmpA, tmpB, AF.Square), reads=[RtB], writes=[RtA])
            P.op("dve", lambda h: h.tensor_scalar(tmpA, tmpA, 0.044715, 1.0, ALU.mult, ALU.add), reads=[RtA], writes=[RtA])
            P.op("dve", lambda h: h.tensor_tensor(tmpA, tmpA, tmpB, ALU.mult), reads=[RtA, RtB], writes=[RtA])
            P.op("act", lambda h: h.activation(tmpA, tmpA, AF.Sigmoid, scale=1.5957691216057308), reads=[RtA], writes=[RtA])
            P.op("dve", lambda h: h.tensor_tensor(dst_ap, tmpA, tmpB, ALU.mult), reads=[RtA, RtB], writes=[Rdst])

        def bc_row(dst, col0, ncols, Rdst, q="sp"):
            src = bass.AP(modrow_d, col0, [[0, 128], [1, ncols]])
            return P.dma(q, dst, src, writes=[Rdst])

        winv = T["w_in"].ap().rearrange("(c p) f -> p c f", p=128)

        cc_evs = []
        RG1 = [Res("G1_%d" % k) for k in range(4)]; RG2 = [Res("G2_%d" % k) for k in range(4)]
        if stop >= 7:
            stg = [AR.view((100 + 16 * i) * KB, [16, 512], BF16) for i in range(3)]
            Rstg = [Res("stg%d" % i) for i in range(3)]
            RSH1 = [Res("SH1_%d" % k) for k in range(4)]; RSH2 = [Res("SH2_%d" % k) for k in range(4)]
            w1s = T["exp_w1"].ap().rearrange("e (c p) f -> e p c f", p=128)
            w2s = T["exp_w2"].ap().rearrange("e (c p) f -> e p c f", p=128)
            rgrp = [list(range(8))]
            n = 0

            def gather(k):
                if NO_CC:
                    return
                cc_evs.append(P.ccop("pool", lambda h, k=k: h.collective_compute(
                    "AllGather", ALU.bypass, replica_groups=rgrp, ins=[SH1[k].ap()], outs=[G1[k].ap()]),
                    reads=[RSH1[k]], writes=[RG1[k]]))
                cc_evs.append(P.ccop("pool", lambda h, k=k: h.collective_compute(
                    "AllGather", ALU.bypass, replica_groups=rgrp, ins=[SH2[k].ap()], outs=[G2[k].ap()]),
                    reads=[RSH2[k]], writes=[RG2[k]]))

            for k in range(4):
                for s8 in range(8):
                    sl, Rs = stg[n % 3], Rstg[n % 3]; n += 1
                    P.dma("pool", sl[:, :, 0:256], w1s[k, :, :, 256 * s8:256 * s8 + 256], writes=[Rs["a"]])
                    P.dma("pool", sl[:, :, 256:512], w1s[k, :, :, D + 256 * s8:D + 256 * s8 + 256], writes=[Rs["b"]])
                    P.dma("sp", SH1[k].ap()[s8 * 128:(s8 + 1) * 128, :], sl.rearrange("p c f -> p (c f)"), reads=[Rs], writes=[RSH1[k][s8]])
                for s4 in range(4):
                    sl, Rs = stg[n % 3], Rstg[n % 3]; n += 1
                    P.dma("pool", sl, w2s[k, :, :, s4 * 512:(s4 + 1) * 512], writes=[Rs])
                    P.dma("sp", SH2[k].ap()[s4 * 128:(s4 + 1) * 128, :], sl.rearrange("p c f -> p (c f)"), reads=[Rs], writes=[RSH2[k][s4]])
                if k >= 1:
                    gather(k - 1)
            gather(3)
            P.barrier(keep=cc_evs)

        L = Bump(AR, 8 * KB, ARENA_KB * KB)
        cf = L([32]); cs_b = L([32], BF16); mb_fm = L([96]); n1g = L([16])
        mrow = [L([512], parts=2) for _ in range(2)]; mbrow = L([6 * D], parts=1)
        mslab = [L([16, 512], BF16) for _ in range(2)]
        Rsl = [Res("mslab%d" % i) for i in range(2)]
        Rc, Rmb, Rn1, Rmbrow = Res("cf"), Res("mb_fm"), Res("n1g"), Res("mbrow")
        Rmrow = [Res("mrow%d" % i) for i in range(2)]
        P.dma("sp", cf, T["cfm"].ap(), writes=[Rc["f"]])
        P.dma("sp", mb_fm, T["modb_fm"].ap(), writes=[Rmb])
        P.dma("sp", n1g, T["n1g_fm"].ap(), writes=[Rn1])
        P.dma("sp", mbrow, T["modb_row"].ap(), writes=[Rmbrow])
        P.op("act", lambda h: h.activation(cs_b, cf, AF.Silu), reads=[Rc["f"]], writes=[Rc["b"]])
        csv = cs_b.rearrange("p (c j) -> p c j", j=2)
        mwv = T["mod_w"].ap().rearrange("(c p) f -> p c f", p=128)
        for s in range(24):
            sl = mslab[s % 2]; R = Rsl[s % 2]
            P.dma("pool", sl, mwv[:, :, s * 512:(s + 1) * 512], writes=[R])
            if s < 8:
                for k in range(4):
                    kk = s * 4 + k
                    for c in range(16):
                        P.op("pe", lambda h, sl=sl, k=k, c=c, kk=kk: h.matmul(
                            PS[0][:, 2 * kk:2 * kk + 2], sl[:, c, k * 128:(k + 1) * 128], csv[:, c, :],
                            start=(c == 0), stop=(c == 15)), reads=[R, Rc["b"]], writes=[RPS[0]])
            if s >= 8:
                pr, Rpr = PS[1 + s % 2], RPS[1 + s % 2]
                mr, Rmr = mrow[s % 2], Rmrow[s % 2]
                for c in range(16):
                    P.op("pe", lambda h, sl=sl, c=c, pr=pr: h.matmul(
                        pr[0:2, :], csv[:, c, :], sl[:, c, :], start=(c == 0), stop=(c == 15)),
                        reads=[R, Rc["b"]], writes=[Rpr])
                P.op("dve", lambda h, s=s, pr=pr, mr=mr: h.tensor_tensor(
                    mr[0:1, :], pr[0:1, :], mbrow[0:1, s * 512:(s + 1) * 512], ALU.add),
                    reads=[Rpr, Rmbrow], writes=[Rmr])
                P.dma("sp", modrow_d.ap()[0:1, s * 512:(s + 1) * 512], mr[0:1, :], reads=[Rmr])
        psv = PS[0][:, 0:64].rearrange("p (k j) -> p k j", j=2)
        P.op("dve", lambda h: h.tensor_tensor(modfm[:, 0:32], psv[:, :, 0], mb_fm[:, 0:32], ALU.add),
             reads=[RPS[0], Rmb], writes=[Rmod["l"]])
        P.op("dve", lambda h: h.tensor_tensor(modfm[:, 32:64], psv[:, :, 1], mb_fm[:, 0:32], ALU.add),
             reads=[RPS[0], Rmb], writes=[Rmod["c"]])
        for j, key in ((0, "l"), (1, "c")):
            P.op("dve", lambda h, j=j: h.scalar_tensor_tensor(
                A1[:, 16 * j:16 * j + 16], modfm[:, 32 * j + 16:32 * j + 32], 1.0, n1g, ALU.add, ALU.mult),
                reads=[Rmod[key], Rn1], writes=[Rmod["A%d" % j]])
            P.op("dve", lambda h, j=j: h.tensor_copy(B1[:, 16 * j:16 * j + 16], modfm[:, 32 * j:32 * j + 16]),
                 reads=[Rmod[key]], writes=[Rmod["B%d" % j]])
        dbg("modfm", modfm, [128, 64], [Rmod])
        P.barrier(keep=cc_evs)

        hT_oth = AR.view(72 * KB, [16, TOWN], BF16); RhT_oth = Res("hT_oth")
        hT_ctx = AR.view(104 * KB, [16, TCTX], BF16); RhT_ctx = Res("hT_ctx")
        G13 = Bump(AR, 112 * KB, 116 * KB)
        WT = G13([H, 2, 18]); FLO = G13([H, 2, 18]); DECbc = G13([H, 2, 18])
        RWT, RFLO, RDEC = Res("WT"), Res("FLO"), Res("DECbc")
        hbufs = {"ctx": (hT_ctx, RhT_ctx), "own": (hT_own, RhT_own), "oth": (hT_oth, RhT_oth)}

        def chunk_h(g):
            if g < 2:
                return hT_ctx, RhT_ctx, g * 128
            if g < 10:
                return hT_own, RhT_own, (g - 2) * 128
            return hT_oth, RhT_oth, (g - 10) * 128

        if stop >= 1:
            L = Bump(AR, 116 * KB, ARENA_KB * KB)
            xs = [L([16, 256]) for _ in range(2)]; Rxs = [Res("xs%d" % i) for i in range(2)]
            sq = L([16, 256], BF16); Rsq = Res("sq")
            rs = L([256]); Rrs = Res("rs")
            tmp = [L([256]) for _ in range(2)]; Rtmp = [Res("p1tmp%d" % i) for i in range(2)]
            groups = [("ctx", 0, 1)] + [("own", i, 0) for i in range(4)] + [("oth", i, 0) for i in range(4)]
            for gi, (nm, i, mj) in enumerate(groups):
                src = T["xT_" + nm].ap().rearrange("(c p) t -> p c t", p=128)
                xb = xs[gi % 2]; Rx = Rxs[gi % 2]
                hb, Rh = hbufs[nm]
                P.dma("sp", xb, src[:, :, i * 256:(i + 1) * 256], writes=[Rx])
                P.op("act", lambda h, xb=xb: h.activation(sq, xb, AF.Square), reads=[Rx], writes=[Rsq])
                for c in range(16):
                    P.op("pe", lambda h, c=c: h.matmul(PS[2][:, 0:256], ones_b, sq[:, c, :], start=(c == 0), stop=(c == 15)),
                         reads=[Rsq, RC["ob"]], writes=[RPS[2]])
                P.op("act", lambda h: h.activation(rs, PS[2][:, 0:256], AF.Sqrt, bias=EPS, scale=1.0 / D),
                     reads=[RPS[2]], writes=[Rrs])
                P.op("dve", lambda h: h.reciprocal(rs, rs), reads=[Rrs], writes=[Rrs])
                for c in range(16):
                    tb = tmp[c % 2]; Rt = Rtmp[c % 2]
                    P.op("dve", lambda h, xb=xb, c=c, tb=tb, mj=mj: h.scalar_tensor_tensor(
                        tb, xb[:, c, :], A1[:, 16 * mj + c:16 * mj + c + 1], rs, ALU.mult, ALU.mult),
                        reads=[Rx, Rrs, Rmod["A%d" % mj]], writes=[Rt])
                    P.op("act", lambda h, hb=hb, c=c, tb=tb, mj=mj, i=i: h.activation(
                        hb[:, c, i * 256:(i + 1) * 256], tb, AF.Identity,
                        bias=B1[:, 16 * mj + c:16 * mj + c + 1], scale=1.0),
                        reads=[Rt, Rmod["B%d" % mj]], writes=[Rh[(c, i)]])
            dbg("hT_own", hT_own, [128, 16, TOWN], [RhT_own], BF16)
            dbg("hT_ctx", hT_ctx, [128, 16, TCTX], [RhT_ctx], BF16)
            P.barrier(keep=cc_evs)

        fwd_order = list(range(0, 10))
        bwd_order = [1, 0] + list(range(17, 1, -1))
        orders = [fwd_order, bwd_order]
        if stop >= 2:
            L = Bump(AR, 116 * KB, ARENA_KB * KB)
            wgb = L([16, 32], BF16); Rwg = Res("wgb")
            gb_bc = L([32]); Rgb = Res("gb_bc")
            G = L([18, 32]); RG = Res("G")
            SP = L([18, 2, 8]); RSP = Res("SP")
            CS = L([18, 2, 8]); RCS = Res("CS")
            AT = L([18, 2, 8]); RAT = Res("AT")
            amax = L([2, 18], parts=8); Ram = Res("amax")
            csend = L([2, 18], parts=8); Rce = Res("csend")
            Q = L([2, 2, 18], parts=8); RQ = Res("Q")
            mcur = L([2], parts=8); Rmc = Res("mcur")
            BD = L([H, 72], parts=8); RBD = Res("BD")
            Mbc = L([H, 2, 2, 18]); RMbc = Res("Mbc")
            tmpw = L([18]); Rtw = Res("tmpw")
            P.dma("pool", wgb, T["wg"].ap().rearrange("(c p) f -> p c f", p=128), writes=[Rwg])
            P.dma("sp", gb_bc, T["gateb_bc"].ap(), writes=[Rgb])
            for g in range(18):
                hb, Rh, off = chunk_h(g)
                pst, Rp = (PS[0], RPS[0]) if g < 16 else (PS[1], RPS[1])
                gg = g if g < 16 else g - 16
                for c in range(16):
                    P.op("pe", lambda h, hb=hb, off=off, c=c, pst=pst, gg=gg: h.matmul(
                        pst[:, gg * 32:(gg + 1) * 32], hb[:, c, off:off + 128], wgb[:, c, :],
                        start=(c == 0), stop=(c == 15)), reads=[Rh, Rwg], writes=[Rp])
                P.op("dve", lambda h, pst=pst, gg=gg, g=g: h.tensor_tensor(
                    G[:, g, :], pst[:, gg * 32:(gg + 1) * 32], gb_bc, ALU.add),
                    reads=[Rp, Rgb], writes=[RG[g]])
            Gv = G.rearrange("p g (a k) -> p g a k", k=8)
            for d in range(2):
                P.op("act", lambda h, d=d: h.activation(SP[:, :, d, :], Gv[:, :, 2 * d + 1, :], AF.Exp, scale=-1.0),
                     reads=[RG], writes=[RSP[d]])
                P.op("act", lambda h, d=d: h.activation(SP[:, :, d, :], SP[:, :, d, :], AF.Ln, bias=1.0, scale=1.0),
                     reads=[RSP[d]], writes=[RSP[d]])
            for g in range(18):
                for d in range(2):
                    o = (g * 2 + d) * 8
                    P.op("pe", lambda h, g=g, d=d, o=o: h.matmul(PS[2][:, o:o + 8], masks[d], SP[:, g, d, :], start=True, stop=True),
                         reads=[RSP, RC["mf"], RC["mb"]], writes=[RPS[2]])
            P.op("act", lambda h: h.copy(CS.rearrange("p g d k -> p (g d k)"), PS[2][:, 0:288]), reads=[RPS[2]], writes=[RCS])
            for d in range(2):
                P.op("dve", lambda h, d=d: h.tensor_tensor(AT[:, :, d, :], CS[:, :, d, :], Gv[:, :, 2 * d, :], ALU.add),
                     reads=[RCS, RG], writes=[RAT[d]])
            for d in range(2):
                for q4 in range(5):
                    gs = list(range(q4 * 4, min(18, q4 * 4 + 4)))
                    pst, Rp = (PS[3], RPS[3]) if q4 % 2 == 0 else (PS[4], RPS[4])
                    for j, g in enumerate(gs):
                        P.op("pe", lambda h, g=g, d=d, j=j, pst=pst: h.matmul(
                            pst[0:8, j * 128:(j + 1) * 128], AT[:, g, d, :], ident_f, start=True, stop=True),
                            reads=[RAT[d], RC["i"]], writes=[Rp])
                    n = len(gs)
                    P.op("dve", lambda h, pst=pst, n=n, d=d, q4=q4: h.tensor_reduce(
                        amax[0:8, d, q4 * 4:q4 * 4 + n], pst[0:8, 0:n * 128].rearrange("p (j t) -> p j t", t=128),
                        AX.X, ALU.max), reads=[Rp], writes=[Ram[(d, q4)]])
            for g in range(18):
                for d in range(2):
                    o = d * 18 + g
                    P.op("pe", lambda h, g=g, d=d, o=o: h.matmul(PS[5][0:8, o:o + 1], SP[:, g, d, :], ones_f[:, 0:1], start=True, stop=True),
                         reads=[RSP, RC["of"]], writes=[RPS[5]])
            P.op("act", lambda h: h.copy(csend.rearrange("p d g -> p (d g)"), PS[5][0:8, 0:36]), reads=[RPS[5]], writes=[Rce])
            P.op("dve", lambda h: h.memset(mcur, 0.0), writes=[Rmc])
            P.op("dve", lambda h: h.memset(Q.rearrange("p a d g -> p (a d g)"), 0.0), writes=[RQ])
            for d in range(2):
                for g in orders[d]:
                    P.op("dve", lambda h, d=d, g=g: h.tensor_tensor(Q[:, 0, d, g:g + 1], mcur[:, d:d + 1], amax[:, d, g:g + 1], ALU.max),
                         reads=[Rmc, Ram], writes=[RQ[("M", d, g)]])
                    P.op("dve", lambda h, d=d, g=g: h.tensor_tensor(Q[:, 1, d, g:g + 1], mcur[:, d:d + 1], Q[:, 0, d, g:g + 1], ALU.subtract),
                         reads=[Rmc, RQ[("M", d, g)]], writes=[RQ[("D", d, g)]])
                    P.op("dve", lambda h, d=d, g=g: h.tensor_tensor(mcur[:, d:d + 1], Q[:, 0, d, g:g + 1], csend[:, d, g:g + 1], ALU.subtract),
                         reads=[RQ[("M", d, g)], Rce], writes=[Rmc])
            Qf = Q.rearrange("p a d g -> p (a d g)")
            for hh in range(H):
                P.op("dve", lambda h, hh=hh: h.tensor_scalar(BD[:, hh, :], Qf, ident_f[0:8, hh:hh + 1], None, ALU.mult),
                     reads=[RQ, RC["i"]], writes=[RBD[hh]])
            for half2 in range(2):
                P.op("pe", lambda h, half2=half2: h.matmul(
                    PS[6][:, 0:288], ones_f[0:8, :], BD[:, half2 * 4:(half2 + 1) * 4, :].rearrange("p h q -> p (h q)"),
                    start=True, stop=True), reads=[RBD, RC["of"]], writes=[RPS[6]])
                P.op("act", lambda h, half2=half2: h.copy(
                    Mbc[:, half2 * 4:(half2 + 1) * 4, :, :, :].rearrange("p h a d g -> p (h a d g)"), PS[6][:, 0:288]),
                    reads=[RPS[6]], writes=[RMbc[half2]])
            for hh in range(H):
                for d in range(2):
                    P.op("dve", lambda h, hh=hh, d=d: h.tensor_tensor(tmpw, AT[:, :, d, hh], Mbc[:, hh, 0, d, :], ALU.subtract),
                         reads=[RAT, RMbc], writes=[Rtw])
                    P.op("act", lambda h, hh=hh, d=d: h.activation(WT[:, hh, d, :], tmpw, AF.Exp), reads=[Rtw], writes=[RWT[(hh, d)]])
                    P.op("dve", lambda h, hh=hh, d=d: h.tensor_tensor(tmpw, CS[:, :, d, hh], Mbc[:, hh, 0, d, :], ALU.subtract),
                         reads=[RCS, RMbc], writes=[Rtw])
                    P.op("act", lambda h, hh=hh, d=d: h.activation(FLO[:, hh, d, :], tmpw, AF.Exp), reads=[Rtw], writes=[RFLO[(hh, d)]])
                    P.op("act", lambda h, hh=hh, d=d: h.activation(DECbc[:, hh, d, :], Mbc[:, hh, 1, d, :], AF.Exp),
                         reads=[RMbc], writes=[RDEC[(hh, d)]])
            dbg("WT", WT, [128, H, 2, 18], [RWT])
            dbg("DECbc", DECbc, [128, H, 2, 18], [RDEC])
            dbg("FLO", FLO, [128, H, 2, 18], [RFLO])
            dbg("G", G, [128, 18, 32], [RG])
            P.barrier(keep=cc_evs)

        if stop >= 3:
            L = Bump(AR, 116 * KB, ARENA_KB * KB)
            WS = L([16, 768], BF16); RWS = Res("WS")
            cw = L([16, 5]); cbv = L([16]); Rcw = Res("cw")
            zq = L([TOWN + 4]); Rzq = Res("zq")
            zk = L([2 * TOWN + 4]); Rzk = Res("zk")
            zkc = L([TCTX + 4]); Rzkc = Res("zkc")
            acq = L([2 * TOWN]); Racq = Res("acq")
            qT = L([TOWN], BF16); RqT = Res("qT")
            kT = L([18 * 128], BF16); RkT = Res("kT")
            ktok = L([18, 128], BF16); Rktok = Res("ktok")
            vtok = L([18, 258], BF16); Rvtok = Res("vtok")
            so = L([8, 256], BF16); Rso = Res("so")
            Hacc = L([8, 256]); RH = Res("Hacc")
            Cst = [L([260]) for _ in range(2)]; RCst = [Res("Cst%d" % d) for d in range(2)]
            Cbf = [L([258], BF16) for _ in range(2)]; RCbf = [Res("Cbf%d" % d) for d in range(2)]
            SpT = [L([128], BF16) for _ in range(2)]; RSpT = [Res("SpT%d" % d) for d in range(2)]
            kw = [L([128], BF16) for _ in range(2)]; Rkw = [Res("kw%d" % d) for d in range(2)]
            rr = [L([1]) for _ in range(2)]; Rrr = [Res("rr%d" % d) for d in range(2)]
            gmb = L([256]); Rgmb = Res("gmb")
            ssh = L([8]); Rssh = Res("ssh")
            rot = [L([256]) for _ in range(2)]; Rrot = [Res("rot%d" % i) for i in range(2)]
            rob = [L([256], BF16) for _ in range(2)]; Rrob = [Res("rob%d" % i) for i in range(2)]
            sqh = acq.rearrange("p (g e) -> p g e", e=256)
            P.dma("sp", cw.rearrange("p c j -> p (c j)"), T["convw_fm"].ap(), writes=[Rcw["w"]])
            P.dma("sp", cbv, T["convb_fm"].ap(), writes=[Rcw["b"]])
            P.op("dve", lambda h: h.memset(vtok[:, :, 256:257], 1.0), writes=[Rvtok["one"]])
            for hh in range(P3_HEADS):
                P.dma("pool", WS[:, :, 0:128], winv[:, :, OFF_Q + hh * 128:OFF_Q + (hh + 1) * 128], writes=[RWS["q"]])
                P.dma("pool", WS[:, :, 128:256], winv[:, :, OFF_K + hh * 128:OFF_K + (hh + 1) * 128], writes=[RWS["k"]])
                P.dma("pool", WS[:, :, 256:512], winv[:, :, OFF_V + hh * 256:OFF_V + (hh + 1) * 256], writes=[RWS["v"]])
                P.dma("pool", WS[:, :, 512:768], winv[:, :, OFF_O + hh * 256:OFF_O + (hh + 1) * 256], writes=[RWS["o"]])
                P.dma("sp", gmb, T["mng_bc"].ap()[:, hh * 256:(hh + 1) * 256], writes=[Rgmb])
                P.op("dve", lambda h: h.memset(zq[:, 0:2], 0.0), writes=[Rzq["h0"]])
                P.op("dve", lambda h: h.memset(zk[:, 0:2], 0.0), writes=[Rzk["h0"]])
                P.op("dve", lambda h: h.memset(zk[:, 2 * TOWN + 2:2 * TOWN + 4], 0.0), writes=[Rzk["h1"]])
                P.op("dve", lambda h: h.memset(zkc[:, 0:2], 0.0), writes=[Rzkc["h0"]])
                P.op("dve", lambda h: h.memset(zkc[:, TCTX + 2:TCTX + 4], 0.0), writes=[Rzkc["h1"]])
                if P3_PART < -2:
                    continue
                pi = 0
                jobs = []
                for tg in range(2):
                    jobs.append((0, hT_own, RhT_own, tg * 512, 512, zq, Rzq, 2 + tg * 512))
                jobs.append((0, hT_oth, RhT_oth, 0, 2, zq, Rzq, 2 + TOWN))
                for tg in range(2):
                    jobs.append((128, hT_own, RhT_own, tg * 512, 512, zk, Rzk, 2 + tg * 512))
                for tg in range(2):
                    jobs.append((128, hT_oth, RhT_oth, tg * 512, 512, zk, Rzk, 2 + TOWN + tg * 512))
                jobs.append((128, hT_ctx, RhT_ctx, 0, 256, zkc, Rzkc, 2))
                for (wo, hb, Rh, toff, n, zb, Rz, zoff) in jobs:
                    pst, Rp = (PS[pi % 2], RPS[pi % 2]); pi += 1
                    for c in range(16):
                        P.op("pe", lambda h, pst=pst, wo=wo, hb=hb, toff=toff, n=n, c=c: h.matmul(
                            pst[:, 0:n], WS[:, c, wo:wo + 128], hb[:, c, toff:toff + n], start=(c == 0), stop=(c == 15)),
                            reads=[Rh, RWS["q" if wo == 0 else "k"]], writes=[Rp])
                    P.op("act", lambda h, pst=pst, zb=zb, zoff=zoff, n=n: h.copy(zb[:, zoff:zoff + n], pst[:, 0:n]),
                         reads=[Rp], writes=[Rz[zoff]])

                def conv(zb, Rz, n, cidx, dst, Rdst, scale, hh=hh):
                    ci = cidx * 8 + hh
                    P.op("dve", lambda h: h.tensor_scalar(acq[:, 0:n], zb[:, 0:n], cw[:, ci, 0:1], cbv[:, ci:ci + 1], ALU.mult, ALU.add),
                         reads=[Rz, Rcw], writes=[Racq])
                    for j in range(1, 5):
                        P.op("dve", lambda h, j=j: h.scalar_tensor_tensor(acq[:, 0:n], zb[:, j:j + n], cw[:, ci, j:j + 1], acq[:, 0:n], ALU.mult, ALU.add),
                             reads=[Rz, Rcw, Racq], writes=[Racq])
                    if scale is None:
                        P.op("act", lambda h: h.activation(dst, acq[:, 0:n], AF.Silu), reads=[Racq], writes=[Rdst])
                    else:
                        P.op("act", lambda h: h.activation(acq[:, 0:n], acq[:, 0:n], AF.Silu), reads=[Racq], writes=[Racq])
                        P.op("dve", lambda h: h.tensor_scalar(dst, acq[:, 0:n], scale, None, ALU.mult), reads=[Racq], writes=[Rdst])

                if P3_PART < -1:
                    continue
                conv(zq, Rzq, TOWN, 0, qT, RqT, 128.0 ** -0.5)
                conv(zk, Rzk, 2 * TOWN, 1, kT[:, 256:256 + 2 * TOWN], RkT["lat"], None)
                conv(zkc, Rzkc, TCTX, 1, kT[:, 0:256], RkT["ctx"], None)
                if hh == 0 and P3_PART >= 0:
                    dbg("qT0", qT, [128, TOWN], [RqT], BF16)
                    dbg("kT0", kT, [128, 18 * 128], [RkT], BF16)
                if P3_PART < 1:
                    continue
                for g0 in ([] if P3_SKIPK else range(0, 18, 4)):
                    gs = list(range(g0, min(18, g0 + 4)))
                    pt, Rpt = PS[6 + (g0 // 4) % 2], RPS[6 + (g0 // 4) % 2]
                    for j, g in enumerate(gs):
                        P.op("pe", lambda h, j=j, g=g, pt=pt: h.matmul(pt[:, j * 128:(j + 1) * 128], kT[:, g * 128:(g + 1) * 128], ident_b, start=True, stop=True),
                             reads=[RkT, RC["ib"]], writes=[Rpt])
                    n = len(gs)
                    P.op("act", lambda h, g0=g0, n=n, pt=pt: h.copy(ktok[:, g0:g0 + n, :].rearrange("p g d -> p (g d)"), pt[:, 0:n * 128]),
                         reads=[Rpt], writes=[Rktok[g0]])
                for g in ([] if P3_SKIPV == 1 else range(18)):
                    hb, Rh, off = chunk_h(g)
                    own = 2 <= g < 10
                    n = 512 if own else 256
                    pst, Rp = (PS[2 + g % 2], RPS[2 + g % 2])
                    for c in range(16):
                        P.op("pe", lambda h, pst=pst, hb=hb, off=off, c=c, n=n: h.matmul(
                            pst[:, 0:n], hb[:, c, off:off + 128], WS[:, c, 256:256 + n], start=(c == 0), stop=(c == 15)),
                            reads=[Rh, RWS["v"], RWS["o"]], writes=[Rp])
                    if P3_SKIPV != 3:
                        P.op("dve", lambda h, pst=pst, g=g: h.tensor_copy(vtok[:, g, 0:256], pst[:, 0:256]), reads=[Rp], writes=[Rvtok[g]])
                    if own and P3_SKIPV != 2:
                        P.op("dve", lambda h, pst=pst: h.tensor_copy(rot[0], pst[:, 256:512]), reads=[Rp], writes=[Rrot[0]])
                        P.op("act", lambda h, g=g: h.activation(so[:, g - 2, :], rot[0], AF.Sigmoid),
                             reads=[Rrot[0]], writes=[Rso[g]])
                if hh == 0:
                    dbg("vtok0", vtok, [128, 18, 258], [Rvtok], BF16)
                if P3_PART < 2:
                    continue
                for d in range(2):
                    P.op("dve", lambda h, d=d: h.memset(Cst[d], 0.0), writes=[RCst[d]])
                first_done = {}
                steps = []
                for i in range(18):
                    for d in range(2):
                        if i < len(orders[d]):
                            steps.append((d, orders[d][i]))
                for (d, g) in steps[:P3_STEPS]:
                    full = 2 <= g < 10
                    gi = g - 2
                    dec = DECbc[:, hh, d, g:g + 1]
                    wv = WT[:, hh, d, g:g + 1]
                    pS, RpS = PS[4], RPS[4]
                    pN, RpN = PS[5 + d], RPS[5 + d]
                    pU, RpU = PS[0 + d], RPS[0 + d]
                    if full:
                        P.op("act", lambda h, d=d, dec=dec: h.activation(Cbf[d][:, 0:257], Cst[d][:, 0:257], AF.Identity, scale=dec),
                             reads=[RCst[d], RDEC], writes=[RCbf[d]])
                        P.op("pe", lambda h, g=g, gi=gi: h.matmul(pS[:, 0:128], kT[:, g * 128:(g + 1) * 128], qT[:, gi * 128:(gi + 1) * 128],
                                                                 start=True, stop=True), reads=[RkT, RqT], writes=[RpS])
                        P.op("dve", lambda h, d=d, wv=wv: h.scalar_tensor_tensor(SpT[d], pS[:, 0:128], wv, masks[d], ALU.mult, ALU.mult),
                             reads=[RpS, RWT, RC["mf"], RC["mb"]], writes=[RSpT[d]])
                        P.op("pe", lambda h, d=d, g=g, pN=pN: h.matmul(pN[:, 0:257], SpT[d], vtok[:, g, 0:257], start=True, stop=False),
                             reads=[RSpT[d], Rvtok], writes=[RpN])
                        P.op("pe", lambda h, d=d, gi=gi, pN=pN: h.matmul(pN[:, 0:257], qT[:, gi * 128:(gi + 1) * 128], Cbf[d][:, 0:257], start=False, stop=True),
                             reads=[RqT, RCbf[d]], writes=[RpN])
                    P.op("act", lambda h, d=d, g=g, wv=wv: h.activation(kw[d], ktok[:, g, :], AF.Identity, scale=wv),
                         reads=[Rktok, RWT], writes=[Rkw[d]])
                    P.op("pe", lambda h, d=d, g=g, pU=pU: h.matmul(pU[:, 0:257], kw[d], vtok[:, g, 0:257], start=True, stop=True),
                         reads=[Rkw[d], Rvtok], writes=[RpU])
                    P.op("dve", lambda h, d=d, dec=dec, pU=pU: h.scalar_tensor_tensor(Cst[d][:, 0:257], Cst[d][:, 0:257], dec, pU[:, 0:257], ALU.mult, ALU.add),
                         reads=[RCst[d], RDEC, RpU, RCbf[d]], writes=[RCst[d]])
                    if full:
                        P.op("dve", lambda h, d=d, pN=pN: h.tensor_copy(rr[d], pN[:, 256:257]), reads=[RpN], writes=[Rrr[d]])
                        P.op("dve", lambda h, d=d: h.scalar_tensor_tensor(rr[d], rr[d], -1.0, rr[d], ALU.mult, ALU.max),
                             reads=[Rrr[d]], writes=[Rrr[d]])
                        P.op("dve", lambda h, d=d, g=g, hh=hh: h.tensor_scalar(rr[d], rr[d], FLO[:, hh, d, g:g + 1], None, ALU.max),
                             reads=[Rrr[d], RFLO], writes=[Rrr[d]])
                        P.op("dve", lambda h, d=d: h.reciprocal(rr[d], rr[d]), reads=[Rrr[d]], writes=[Rrr[d]])
                        if gi not in first_done:
                            first_done[gi] = True
                            P.op("dve", lambda h, d=d, gi=gi, pN=pN: h.tensor_scalar(Hacc[:, gi, :], pN[:, 0:256], rr[d][:, 0:1], None, ALU.mult),
                                 reads=[RpN, Rrr[d]], writes=[RH[gi]])
                        else:
                            P.op("dve", lambda h, d=d, gi=gi, pN=pN: h.scalar_tensor_tensor(Hacc[:, gi, :], pN[:, 0:256], rr[d][:, 0:1], Hacc[:, gi, :], ALU.mult, ALU.add),
                                 reads=[RpN, Rrr[d], RH[gi]], writes=[RH[gi]])
                if hh == 0:
                    dbg("Hacc0", Hacc, [128, 8, 256], [RH])
                if P3_PART < 3:
                    continue
                P.op("dve", lambda h: h.tensor_tensor(sqh, Hacc, Hacc, ALU.mult), reads=[RH], writes=[Racq])
                P.op("dve", lambda h: h.tensor_reduce(ssh, sqh, AX.X, ALU.add), reads=[Racq], writes=[Rssh])
                P.op("act", lambda h: h.activation(ssh, ssh, AF.Sqrt, bias=EPS, scale=1.0 / 256.0), reads=[Rssh], writes=[Rssh])
                P.op("dve", lambda h: h.reciprocal(ssh, ssh), reads=[Rssh], writes=[Rssh])
                for gi in range(8):
                    rt, Rrt = rot[gi % 2], Rrot[gi % 2]
                    rb, Rrb = rob[gi % 2], Rrob[gi % 2]
                    P.op("dve", lambda h, gi=gi, rt=rt: h.scalar_tensor_tensor(rt, Hacc[:, gi, :], ssh[:, gi:gi + 1], gmb, ALU.mult, ALU.mult),
                         reads=[RH, Rssh, Rgmb], writes=[Rrt])
                    P.op("dve", lambda h, gi=gi, rt=rt, rb=rb: h.tensor_tensor(rb, rt, so[:, gi, :], ALU.mult), reads=[Rrt, Rso], writes=[Rrb])
                    pt, Rpt = PS[6 + gi % 2], RPS[6 + gi % 2]
                    for e2 in range(2):
                        P.op("pe", lambda h, rb=rb, e2=e2, pt=pt: h.matmul(pt[:, e2 * 128:(e2 + 1) * 128], rb[:, e2 * 128:(e2 + 1) * 128], ident_b, start=True, stop=True),
                             reads=[Rrb, RC["ib"]], writes=[Rpt])
                    P.op("act", lambda h, gi=gi, hh=hh, pt=pt: h.copy(
                        ROT[:, 2 * hh:2 * hh + 2, gi * 128:(gi + 1) * 128], pt[:, 0:256].rearrange("p (e t) -> p e t", t=128)),
                        reads=[Rpt], writes=[RBB[(hh, gi)]])
            dbg("ROT", ROT, [128, 16, TOWN], [RBB], BF16)
            P.barrier(keep=cc_evs)

        NSL = 3
        slab_box = {}

        def set_slabs(base):
            slab_box["s"] = [AR.view(base + i * 16 * KB, [16, 512], BF16) for i in range(NSL)]
            slab_box["R"] = [Res("slab%d_%d" % (base, i)) for i in range(NSL)]
            slab_box["n"] = 0

        def load_slab(pieces):
            i = slab_box["n"] % NSL
            slab_box["n"] += 1
            for ap, off, n in pieces:
                P.dma("pool", slab_box["s"][i][:, :, off:off + n], ap, writes=[slab_box["R"][i][off]])
            return slab_box["s"][i], slab_box["R"][i]

        def wslab(dt, col0, n=512):
            v = dt.ap().rearrange("(c p) f -> p c f", p=128)
            return [(v[:, :, col0:col0 + n], 0, n)]

        set_slabs(104 * KB)
        TT = Bump(AR, 152 * KB, 160 * KB)
        tA = [TT([512]) for _ in range(2)]; tB = [TT([512]) for _ in range(2)]
        RtA = [Res("tA%d" % i) for i in range(2)]; RtB = [Res("tB%d" % i) for i in range(2)]

        def proj_and_gate(rhs_buf, Rrhs, wt, mg_col0, accumulate):
            u = 0
            for s4 in range(4):
                sw, Rw = load_slab(wslab(wt, s4 * 512))
                sg, Rg = load_slab([(winv[:, :, mg_col0 + s4 * 512:mg_col0 + (s4 + 1) * 512], 0, 512)])
                for k in range(4):
                    j = s4 * 4 + k
                    for tg in range(2):
                        py, Rpy = PS[(u % 2) * 2], RPS[(u % 2) * 2]
                        pg, Rpg = PS[(u % 2) * 2 + 1], RPS[(u % 2) * 2 + 1]
                        ta, Rta = tA[u % 2], RtA[u % 2]
                        tb, Rtb = tB[u % 2], RtB[u % 2]
                        u += 1
                        for c in range(16):
                            P.op("pe", lambda h, py=py, sw=sw, k=k, c=c, tg=tg: h.matmul(
                                py, sw[:, c, k * 128:(k + 1) * 128], rhs_buf[:, c, tg * 512:(tg + 1) * 512],
                                start=(c == 0), stop=(c == 15)), reads=[Rw, Rrhs], writes=[Rpy])
                        for c in range(16):
                            P.op("pe", lambda h, pg=pg, sg=sg, k=k, c=c, tg=tg: h.matmul(
                                pg, sg[:, c, k * 128:(k + 1) * 128], hT_own[:, c, tg * 512:(tg + 1) * 512],
                                start=(c == 0), stop=(c == 15)), reads=[Rg, RhT_own], writes=[Rpg])
                        P.op("dve", lambda h, pg=pg, ta=ta: h.tensor_copy(ta, pg), reads=[Rpg], writes=[Rta])
                        P.op("act", lambda h, ta=ta: h.activation(ta, ta, AF.Sigmoid), reads=[Rta], writes=[Rta])
                        dst = MA[:, j, tg * 512:(tg + 1) * 512]
                        if not accumulate:
                            P.op("dve", lambda h, py=py, ta=ta, dst=dst: h.tensor_tensor(dst, ta, py, ALU.mult),
                                 reads=[Rta, Rpy], writes=[RBC[(j, tg)]])
                        else:
                            P.op("dve", lambda h, py=py, ta=ta, tb=tb: h.tensor_tensor(tb, ta, py, ALU.mult),
                                 reads=[Rta, Rpy], writes=[Rtb])
                            P.op("dve", lambda h, tb=tb, dst=dst: h.tensor_tensor(dst, tb, dst, ALU.add),
                                 reads=[Rtb, RBC[(j, tg)]], writes=[RBC[(j, tg)]])

        if stop >= 4:
            proj_and_gate(ROT, RBB, T["proj_a"], OFF_MG, False)
            dbg("MA_a", MA, [128, 16, TOWN], [RBC], BF16)
            P.barrier(keep=cc_evs)

        if stop >= 5:
            UG = ROT
            L = Bump(AR, 160 * KB, ARENA_KB * KB)
            SVG = L([8, D], BF16); RSVG = Res("SVG")
            sng = L([D]); Rsng = Res("sng")
            sgw = L([8, 128], BF16); Rsgw = Res("sgw")
            tC = L([512]); RtC = Res("tC")
            ssp = L([8, 4]); Rssp = Res("ssp")
            ssv = L([8]); Rssv = Res("ssv")
            P.dma("sp", sng, T["sng_bc"].ap(), writes=[Rsng])
            for i in range(2):
                P.dma("sp", tA[i], T["sgwT"].ap()[:, i * 512:(i + 1) * 512], writes=[RtA[i]])
                P.op("dve", lambda h, i=i, src=tA[i]: h.tensor_copy(sgw[:, 4 * i:4 * i + 4, :].rearrange("p g t -> p (g t)"), src),
                     reads=[RtA[i]], writes=[Rsgw[i]])
                P.dma("sp", tB[i][0:1, :], T["sgb_row"].ap()[0:1, i * 512:(i + 1) * 512], writes=[RtB[i]])
                P.op("dve", lambda h, i=i, src=tB[i][0:1, :]: h.tensor_copy(sgb[0:1, 4 * i:4 * i + 4, :].rearrange("p g t -> p (g t)"), src),
                     reads=[RtB[i]], writes=[Rsgb[i]])
            u = 0
            for s4 in range(4):
                sw, Rw = load_slab([(winv[:, :, OFF_SV + s4 * 512:OFF_SV + (s4 + 1) * 512], 0, 512)])
                for gi in range(8):
                    pst, Rp = PS[u % 2], RPS[u % 2]
                    ta, Rta = tA[u % 2], RtA[u % 2]
                    tb, Rtb = tB[u % 2], RtB[u % 2]
                    u += 1
                    for c in range(16):
                        P.op("pe", lambda h, pst=pst, sw=sw, c=c, gi=gi: h.matmul(
                            pst, hT_own[:, c, gi * 128:(gi + 1) * 128], sw[:, c, :], start=(c == 0), stop=(c == 15)),
                            reads=[Rw, RhT_own], writes=[Rp])
                    gelu_tile(pst, Rp, tC, RtC, ta, tb, Rta, Rtb)
                    P.op("dve", lambda h, ta=ta: h.tensor_tensor(ta, tC, tC, ALU.mult), reads=[RtC], writes=[Rta])
                    P.op("dve", lambda h, gi=gi, s4=s4, ta=ta: h.tensor_reduce(ssp[:, gi, s4:s4 + 1], ta, AX.X, ALU.add),
                         reads=[Rta], writes=[Rssp[(gi, s4)]])
                    P.op("act", lambda h, gi=gi, s4=s4: h.copy(SVG[:, gi, s4 * 512:(s4 + 1) * 512], tC),
                         reads=[RtC], writes=[RSVG[(gi, s4)]])
            P.op("dve", lambda h: h.tensor_reduce(ssv, ssp, AX.X, ALU.add), reads=[Rssp], writes=[Rssv])
            P.op("act", lambda h: h.activation(ssv, ssv, AF.Sqrt, bias=EPS, scale=1.0 / D), reads=[Rssv], writes=[Rssv])
            P.op("dve", lambda h: h.reciprocal(ssv, ssv), reads=[Rssv], writes=[Rssv])
            for gi in range(8):
                for s4 in range(4):
                    P.op("dve", lambda h, gi=gi, s4=s4: h.scalar_tensor_tensor(
                        SVG[:, gi, s4 * 512:(s4 + 1) * 512], SVG[:, gi, s4 * 512:(s4 + 1) * 512], ssv[:, gi:gi + 1],
                        sng[:, s4 * 512:(s4 + 1) * 512], ALU.mult, ALU.mult),
                        reads=[RSVG[(gi, s4)], Rssv, Rsng], writes=[RSVG[(gi, s4)]])
            u = 0
            for s4 in range(4):
                sw, Rw = load_slab([(winv[:, :, OFF_U + s4 * 512:OFF_U + (s4 + 1) * 512], 0, 512)])
                for k in range(4):
                    j = s4 * 4 + k
                    for tg in range(2):
                        pst, Rp = PS[u % 2], RPS[u % 2]
                        ta, Rta = tA[u % 2], RtA[u % 2]
                        tb, Rtb = tB[u % 2], RtB[u % 2]
                        u += 1
                        for c in range(16):
                            P.op("pe", lambda h, pst=pst, sw=sw, k=k, c=c, tg=tg: h.matmul(
                                pst, sw[:, c, k * 128:(k + 1) * 128], hT_own[:, c, tg * 512:(tg + 1) * 512],
                                start=(c == 0), stop=(c == 15)), reads=[Rw, RhT_own], writes=[Rp])
                        gelu_tile(pst, Rp, UG[:, j, tg * 512:(tg + 1) * 512], RBB[(j, tg)], ta, tb, Rta, Rtb)
            u = 0
            for gi in range(8):
                for j4 in range(4):
                    pst, Rp = PS[2 + u % 2], RPS[2 + u % 2]
                    u += 1
                    for k in range(4):
                        j = j4 * 4 + k
                        g = j // 2
                        P.op("pe", lambda h, pst=pst, k=k, j=j, g=g, gi=gi: h.matmul(
                            pst[:, k * 128:(k + 1) * 128], SVG[:, gi, j * 128:(j + 1) * 128], sgw[:, g, :], start=True, stop=False),
                            reads=[RSVG, Rsgw], writes=[Rp])
                        P.op("pe", lambda h, pst=pst, k=k, g=g: h.matmul(
                            pst[:, k * 128:(k + 1) * 128], ones_b[0:1, :], sgb[0:1, g, :], start=False, stop=True),
                            reads=[Rsgb, RC["ob"]], writes=[Rp])
                    dst = UG[:, j4 * 4:(j4 + 1) * 4, gi * 128:(gi + 1) * 128]
                    P.op("dve", lambda h, pst=pst, dst=dst: h.tensor_tensor(
                        dst, dst, pst.rearrange("p (k t) -> p k t", t=128), ALU.mult),
                        reads=[Rp, RBB], writes=[RBB[("prd", gi, j4)]])
            dbg("PRD", UG, [128, 16, TOWN], [RBB], BF16)
            proj_and_gate(UG, RBB, T["proj_b"], OFF_MG + D, True)
            dbg("MRG", MA, [128, 16, TOWN], [RBC], BF16)
            P.barrier(keep=cc_evs)

        H2T = hT_own
        RH2T = RBA
        if stop >= 6:
            X1a = AR.view(BB_OFF, [4, D], F32)
            X1b = AR.view(160 * KB, [4, D], F32)
            RX1 = Res("X1")

            def X1(gi):
                return (X1a if gi < 4 else X1b)[:, gi % 4, :]

            bcA = AR.view(136 * KB, [D], F32); bcB = AR.view(144 * KB, [D], F32)
            RbcA, RbcB = Res("bcA"), Res("bcB")
            L = Bump(AR, 192 * KB, ARENA_KB * KB)
            xin = [L([512]) for _ in range(2)]; Rxin = [Res("xin%d" % i) for i in range(2)]
            rwf = L([16, 32]); Rrwf = Res("rwf")
            rb_bc = L([32]); Rrb = Res("rb_bc")
            ss2 = L([8, 4]); Rss2 = Res("ss2")
            rs2 = L([8]); Rrs2 = Res("rs2")
            h2f = [L([128]) for _ in range(2)]; Rh2f = [Res("h2f%d" % i) for i in range(2)]
            LG = L([8, 32]); RLG = Res("LG")
            m8 = L([8]); Rm8 = Res("m8")
            nmx = L([1]); Rnmx = Res("nmx")
            ex = L([32]); Rex = Res("ex")
            sel = L([32]); Rsel = Res("sel")
            ssum = L([1]); Rssum = Res("ssum")
            NSL6 = 2
            bc_row(bcA, 2 * D, D, RbcA)
            P.dma("sp", rwf, T["router_w"].ap().rearrange("(c p) e -> p c e", p=128), writes=[Rrwf])
            P.dma("sp", rb_bc, T["routerb_bc"].ap(), writes=[Rrb])
            u = 0
            for s4 in range(4):
                i6 = s4 % NSL6
                sw, Rw = slab_box["s"][i6], slab_box["R"][i6]
                P.dma("pool", sw, T["w_out"].ap().rearrange("(c p) f -> p c f", p=128)[:, :, s4 * 512:(s4 + 1) * 512], writes=[Rw])
                for gi in range(8):
                    pst, Rp = PS[u % 2], RPS[u % 2]
                    xi, Rxi = xin[u % 2], Rxin[u % 2]
                    ta, Rta = tA[u % 2], RtA[u % 2]
                    u += 1
                    P.dma("sp", xi, T["x_own"].ap()[gi * 128:(gi + 1) * 128, s4 * 512:(s4 + 1) * 512], writes=[Rxi])
                    for c in range(16):
                        P.op("pe", lambda h, pst=pst, sw=sw, c=c, gi=gi: h.matmul(
                            pst, MA[:, c, gi * 128:(gi + 1) * 128], sw[:, c, :], start=(c == 0), stop=(c == 15)),
                            reads=[Rw, RBC], writes=[Rp])
                    dst = X1(gi)[:, s4 * 512:(s4 + 1) * 512]
                    P.op("dve", lambda h, pst=pst, s4=s4, ta=ta: h.tensor_tensor(ta, pst, bcA[:, s4 * 512:(s4 + 1) * 512], ALU.mult),
                         reads=[Rp, RbcA], writes=[Rta])
                    P.op("dve", lambda h, ta=ta, xi=xi, dst=dst: h.tensor_tensor(dst, ta, xi, ALU.add),
                         reads=[Rta, Rxi], writes=[RX1[(gi, s4)]])
                    P.op("act", lambda h, ta=ta, dst=dst: h.activation(ta, dst, AF.Square), reads=[RX1[(gi, s4)]], writes=[Rta])
                    P.op("dve", lambda h, ta=ta, gi=gi, s4=s4: h.tensor_reduce(ss2[:, gi, s4:s4 + 1], ta, AX.X, ALU.add),
                         reads=[Rta], writes=[Rss2[(gi, s4)]])
            for gi in range(8):
                P.dma("sp", x1_d.ap()[gi * 128:(gi + 1) * 128, :], X1(gi), reads=[RX1])
            dbg("X1a", X1a, [128, 4, D], [RX1])
            P.op("dve", lambda h: h.tensor_reduce(rs2, ss2, AX.X, ALU.add), reads=[Rss2], writes=[Rrs2])
            P.op("act", lambda h: h.activation(rs2, rs2, AF.Sqrt, bias=EPS, scale=1.0 / D), reads=[Rrs2], writes=[Rrs2])
            P.op("dve", lambda h: h.reciprocal(rs2, rs2), reads=[Rrs2], writes=[Rrs2])
            P.dma("sp", bcA, T["n2g_bc"].ap(), writes=[RbcA])
            bc_row(bcB, 4 * D, D, RbcB)
            P.op("dve", lambda h: h.scalar_tensor_tensor(bcB, bcB, 1.0, bcA, ALU.add, ALU.mult), reads=[RbcA, RbcB], writes=[RbcB])
            bc_row(bcA, 3 * D, D, RbcA)
            for gi in range(8):
                for s4 in range(4):
                    dst = X1(gi)[:, s4 * 512:(s4 + 1) * 512]
                    P.op("dve", lambda h, dst=dst, gi=gi, s4=s4: h.scalar_tensor_tensor(
                        dst, dst, rs2[:, gi:gi + 1], bcB[:, s4 * 512:(s4 + 1) * 512], ALU.mult, ALU.mult),
                        reads=[RX1[(gi, s4)], Rrs2, RbcB], writes=[RX1[(gi, s4)]])
                    P.op("dve", lambda h, dst=dst, s4=s4: h.tensor_tensor(dst, dst, bcA[:, s4 * 512:(s4 + 1) * 512], ALU.add),
                         reads=[RX1[(gi, s4)], RbcA], writes=[RX1[(gi, s4)]])
            dbg("H2a", X1a, [128, 4, D], [RX1])
            u = 0
            for gi in range(8):
                for c in range(16):
                    pst, Rp = PS[2 + u % 2], RPS[2 + u % 2]
                    hf, Rhf = h2f[u % 2], Rh2f[u % 2]
                    u += 1
                    P.op("pe", lambda h, pst=pst, gi=gi, c=c: h.matmul(pst[:, 0:128], X1(gi)[:, c * 128:(c + 1) * 128], ident_f, start=True, stop=True),
                         reads=[RX1, RC["i"]], writes=[Rp])
                    P.op("act", lambda h, pst=pst, hf=hf: h.copy(hf, pst[:, 0:128]), reads=[Rp], writes=[Rhf])
                    P.op("dve", lambda h, pst=pst, gi=gi, c=c: h.tensor_copy(H2T[:, c, gi * 128:(gi + 1) * 128], pst[:, 0:128]),
                         reads=[Rp], writes=[RH2T[(c, gi)]])
                    P.op("pe", lambda h, hf=hf, gi=gi, c=c: h.matmul(PS[4][:, gi * 32:(gi + 1) * 32], hf, rwf[:, c, :], start=(c == 0), stop=(c == 15)),
                         reads=[Rhf, Rrwf], writes=[RPS[4]])
                P.op("dve", lambda h, gi=gi: h.tensor_tensor(LG[:, gi, :], PS[4][:, gi * 32:(gi + 1) * 32], rb_bc, ALU.add),
                     reads=[RPS[4], Rrb], writes=[RLG[gi]])
                P.op("dve", lambda h, gi=gi: h.max(m8, LG[:, gi, :]), reads=[RLG[gi]], writes=[Rm8])
                P.op("dve", lambda h: h.tensor_scalar(nmx, m8[:, 0:1], -1.0, None, ALU.mult), reads=[Rm8], writes=[Rnmx])
                P.op("act", lambda h, gi=gi: h.activation(ex, LG[:, gi, :], AF.Exp, bias=nmx[:, 0:1], scale=1.0),
                     reads=[RLG[gi], Rnmx], writes=[Rex])
                P.op("dve", lambda h, gi=gi: h.tensor_scalar(sel, LG[:, gi, :], m8[:, 3:4], None, ALU.is_ge),
                     reads=[RLG[gi], Rm8], writes=[Rsel])
                P.op("dve", lambda h: h.tensor_tensor(ex, ex, sel, ALU.mult), reads=[Rex, Rsel], writes=[Rex])
                P.op("dve", lambda h: h.tensor_reduce(ssum, ex, AX.X, ALU.add), reads=[Rex], writes=[Rssum])
                P.op("dve", lambda h: h.reciprocal(ssum, ssum), reads=[Rssum], writes=[Rssum])
                P.op("dve", lambda h, gi=gi: h.tensor_scalar(GT[:, gi, :], ex, ssum[:, 0:1], None, ALU.mult),
                     reads=[Rex, Rssum], writes=[RGT[gi]])
            dbg("GT", GT, [128, 8, 32], [RGT])
            dbg("LG", LG, [128, 8, 32], [RLG])
            dbg("H2T", H2T, [128, 16, TOWN], [RH2T], BF16)
            P.barrier(keep=cc_evs)

        ACC = AR.view(72 * KB, [8, D], F32); RACC = Res("ACC")
        if stop >= 7:
            ACTT = ROT; RACT = RBB
            set_slabs(136 * KB)
            TT = Bump(AR, 184 * KB, 200 * KB)
            tA = [TT([512]) for _ in range(2)]; tB = [TT([512]) for _ in range(2)]; gA = [TT([512]) for _ in range(2)]
            b1 = TT([NE, 32])
            RtA = [Res("m_tA%d" % i) for i in range(2)]; RtB = [Res("m_tB%d" % i) for i in range(2)]
            RgA = [Res("m_gA%d" % i) for i in range(2)]; Rb1 = Res("b1")
            b2 = AR.view(184 * KB, [D], F32, parts=NE); Rb2 = Res("b2")
            GTT = AR.view(192 * KB, [TOWN], F32, parts=NE); RGTT = Res("GTT")
            P.dma("sp", b1.rearrange("p e k -> p (e k)"), T["b1_fm"].ap(), writes=[Rb1])
            P.dma("sp", b2, T["exp_b2"].ap(), writes=[Rb2])
            for gi in range(8):
                P.op("pe", lambda h, gi=gi: h.matmul(PS[6][0:NE, (gi % 4) * 128:(gi % 4 + 1) * 128], GT[:, gi, :], ident_f, start=True, stop=True),
                     reads=[RGT, RC["i"]], writes=[RPS[6]])
                if gi % 4 == 3:
                    P.op("act", lambda h, gi=gi: h.copy(GTT[:, (gi // 4) * 512:(gi // 4 + 1) * 512], PS[6][0:NE, :]),
                         reads=[RPS[6]], writes=[RGTT[gi // 4]])
            u = 0
            for gi in range(8):
                for s4 in range(4):
                    pst, Rp = PS[u % 2], RPS[u % 2]
                    u += 1
                    P.op("pe", lambda h, pst=pst, gi=gi, s4=s4: h.matmul(pst, GTT[:, gi * 128:(gi + 1) * 128], b2[:, s4 * 512:(s4 + 1) * 512], start=True, stop=True),
                         reads=[RGTT, Rb2], writes=[Rp])
                    P.op("act", lambda h, pst=pst, gi=gi, s4=s4: h.copy(ACC[:, gi, s4 * 512:(s4 + 1) * 512], pst),
                         reads=[Rp], writes=[RACC[(gi, s4)]])
            P.barrier()
            u = 0
            ne_run = NE if N_EXPERTS_RUN is None else N_EXPERTS_RUN
            elist = [(k, r) for k in range(4) for r in range(8)][:ne_run]

            def load_gslab(Gt, RGk, k, row0):
                i = slab_box["n"] % NSL
                slab_box["n"] += 1
                P.dma("sp", slab_box["s"][i].rearrange("p c f -> p (c f)"), Gt[k].ap()[row0:row0 + 128, :],
                      reads=[RGk], writes=[slab_box["R"][i]])
                return slab_box["s"][i], slab_box["R"][i]

            for (k, r) in elist:
                e = 4 * r + k
                for s8 in range(8):
                    sw, Rw = load_gslab(G1, RG1[k], k, r * 1024 + s8 * 128)
                    for jj in range(2):
                        f = 2 * s8 + jj
                        for tg in range(2):
                            pg, Rpg = PS[(u % 2) * 2], RPS[(u % 2) * 2]
                            pl, Rpl = PS[(u % 2) * 2 + 1], RPS[(u % 2) * 2 + 1]
                            ta, Rta = tA[u % 2], RtA[u % 2]
                            tb, Rtb = tB[u % 2], RtB[u % 2]
                            ga, Rga = gA[u % 2], RgA[u % 2]
                            u += 1
                            for c in range(16):
                                P.op("pe", lambda h, pg=pg, sw=sw, jj=jj, c=c, tg=tg: h.matmul(
                                    pg, sw[:, c, jj * 128:(jj + 1) * 128], H2T[:, c, tg * 512:(tg + 1) * 512],
                                    start=(c == 0), stop=(c == 15)), reads=[Rw, RH2T], writes=[Rpg])
                            for c in range(16):
                                P.op("pe", lambda h, pl=pl, sw=sw, jj=jj, c=c, tg=tg: h.matmul(
                                    pl, sw[:, c, 256 + jj * 128:256 + (jj + 1) * 128], H2T[:, c, tg * 512:(tg + 1) * 512],
                                    start=(c == 0), stop=(c == 15)), reads=[Rw, RH2T], writes=[Rpl])
                            P.op("dve", lambda h, pg=pg, ga=ga, e=e, f=f: h.tensor_scalar(ga, pg, b1[:, e, f:f + 1], 7.0, ALU.add, ALU.min),
                                 reads=[Rpg, Rb1], writes=[Rga])
                            P.op("act", lambda h, ga=ga, ta=ta: h.activation(ta, ga, AF.Sigmoid, scale=1.702), reads=[Rga], writes=[Rta])
                            P.op("dve", lambda h, pl=pl, tb=tb, e=e, f=f: h.tensor_scalar(tb, pl, b1[:, e, 16 + f:16 + f + 1], 7.0, ALU.add, ALU.min),
                                 reads=[Rpl, Rb1], writes=[Rtb])
                            P.op("dve", lambda h, tb=tb: h.tensor_scalar(tb, tb, -7.0, 1.0, ALU.max, ALU.add), reads=[Rtb], writes=[Rtb])
                            P.op("dve", lambda h, ga=ga, ta=ta: h.tensor_tensor(ga, ga, ta, ALU.mult), reads=[Rga, Rta], writes=[Rga])
                            P.op("dve", lambda h, ga=ga, tb=tb, f=f, tg=tg: h.tensor_tensor(ACTT[:, f, tg * 512:(tg + 1) * 512], ga, tb, ALU.mult),
                                 reads=[Rga, Rtb], writes=[RACT[(f, tg)]])
                for s4 in range(4):
                    sw, Rw = load_gslab(G2, RG2[k], k, r * 512 + s4 * 128)
                    for gi in range(8):
                        pst, Rp = PS[4 + gi % 2], RPS[4 + gi % 2]
                        for c in range(16):
                            P.op("pe", lambda h, pst=pst, sw=sw, c=c, gi=gi: h.matmul(
                                pst, ACTT[:, c, gi * 128:(gi + 1) * 128], sw[:, c, :], start=(c == 0), stop=(c == 15)),
                                reads=[Rw, RACT], writes=[Rp])
                        dst = ACC[:, gi, s4 * 512:(s4 + 1) * 512]
                        P.op("dve", lambda h, pst=pst, dst=dst, gi=gi, e=e: h.scalar_tensor_tensor(dst, pst, GT[:, gi, e:e + 1], dst, ALU.mult, ALU.add),
                             reads=[Rp, RGT, RACC[(gi, s4)]], writes=[RACC[(gi, s4)]])
            dbg("ACCa", ACC[:, 0:4, :], [128, 4, D], [RACC])
            P.barrier(keep=cc_evs)

        if stop >= 8:
            L = Bump(AR, 136 * KB, 184 * KB)
            g5 = L([D]); fg = L([D]); Rg5 = Res("g5"); Rfg = Res("fg")
            x1r = [L([D]) for _ in range(2)]; Rx1r = [Res("x1r%d" % i) for i in range(2)]
            junk = L([D]); Rjunk = Res("junk")
            ssf = L([8]); Rssf = Res("ssf")
            bc_row(g5, 5 * D, D, Rg5)
            P.dma("sp", fg, T["fing_bc"].ap(), writes=[Rfg])
            for gi in range(8):
                xr, Rxr = x1r[gi % 2], Rx1r[gi % 2]
                P.dma("sp", xr, x1_d.ap()[gi * 128:(gi + 1) * 128, :], writes=[Rxr])
                P.op("dve", lambda h, gi=gi: h.tensor_tensor(ACC[:, gi, :], ACC[:, gi, :], g5, ALU.mult), reads=[RACC, Rg5], writes=[RACC[("f", gi)]])
                P.op("dve", lambda h, gi=gi, xr=xr: h.tensor_tensor(ACC[:, gi, :], ACC[:, gi, :], xr, ALU.add), reads=[RACC[("f", gi)], Rxr], writes=[RACC[("f", gi)]])
                P.op("act", lambda h, gi=gi: h.activation(junk, ACC[:, gi, :], AF.Square), reads=[RACC[("f", gi)]], writes=[Rjunk])
                P.op("dve", lambda h, gi=gi: h.tensor_reduce(ssf[:, gi:gi + 1], junk, AX.X, ALU.add), reads=[Rjunk], writes=[Rssf[gi]])
            P.op("act", lambda h: h.activation(ssf, ssf, AF.Sqrt, bias=EPS, scale=1.0 / D), reads=[Rssf], writes=[Rssf])
            P.op("dve", lambda h: h.reciprocal(ssf, ssf), reads=[Rssf], writes=[Rssf])
            for gi in range(8):
                P.op("dve", lambda h, gi=gi: h.scalar_tensor_tensor(ACC[:, gi, :], ACC[:, gi, :], ssf[:, gi:gi + 1], fg, ALU.mult, ALU.mult),
                     reads=[RACC[("f", gi)], Rssf, Rfg], writes=[RACC[("o", gi)]])
                P.dma("sp", out_d.ap()[gi * 128:(gi + 1) * 128, :], ACC[:, gi, :], reads=[RACC[("o", gi)]], is_output=True)
        else:
            zt = AR.view(100 * KB, [D], F32)
            Rzt = Res("zt")
            P.op("dve", lambda h: h.memset(zt, 0.0), writes=[Rzt])
            P.dma("sp", out_d.ap()[0:128, :], zt, reads=[Rzt], is_output=True)
        P.emit()
    return nc, dbg_out


def _fm(v, nch):
    return np.ascontiguousarray(np.asarray(v, np.float32).reshape(nch, 128).T)


def _bc(v):
    return np.ascontiguousarray(np.broadcast_to(np.asarray(v, np.float32)[None, :], (128, v.shape[0])))


def prepare_inputs(x, c, ctx, c_ctx, mod_w, mod_b, norm1_g, norm2_g, w_in, gate_b, conv_w, conv_b,
                   mlstm_norm_g, sgu_norm_g, sgu_w, sgu_b, proj_a, proj_b, w_out, router_w, router_b,
                   exp_w1, exp_b1, exp_w2, exp_b2, final_g):
    f32 = np.float32
    shared = {
        "mod_w": np.ascontiguousarray(mod_w[0], f32), "modb_fm": _fm(mod_b[0], 96),
        "modb_row": np.ascontiguousarray(mod_b[0][None, :], f32),
        "n1g_fm": _fm(norm1_g[0], 16), "n2g_bc": _bc(norm2_g[0]), "fing_bc": _bc(final_g),
        "w_in": np.ascontiguousarray(w_in[0], f32), "convb_fm": _fm(conv_b[0], 16),
        "mng_bc": _bc(mlstm_norm_g[0]), "sng_bc": _bc(sgu_norm_g[0]),
        "proj_a": np.ascontiguousarray(proj_a[0], f32), "proj_b": np.ascontiguousarray(proj_b[0], f32),
        "w_out": np.ascontiguousarray(w_out[0], f32),
        "router_w": np.ascontiguousarray(router_w[0], f32), "routerb_bc": _bc(router_b[0]),
        "b1_fm": np.ascontiguousarray(exp_b1[0].reshape(NE, 32, 128).transpose(2, 0, 1).reshape(128, NE * 32), f32),
        "exp_b2": np.ascontiguousarray(exp_b2[0], f32),
        "ident": np.eye(128, dtype=f32),
        "maskf": np.triu(np.ones((128, 128), f32)),
        "maskb": np.tril(np.ones((128, 128), f32)),
    }
    wg_nat = w_in[0][:, OFF_G:OFF_G + 32]
    in_maps = []
    for core in range(8):
        b, half = core // 2, core % 2
        own = x[b, half * TOWN:(half + 1) * TOWN]
        oth = x[b, (1 - half) * TOWN:(2 - half) * TOWN]
        cx = ctx[b]
        cwl = conv_w[0]
        sgw = sgu_w[0]
        sgb = sgu_b[0]
        if half == 0:
            perm = np.arange(32)
        else:
            own, oth, cx = own[::-1], oth[::-1], cx[::-1]
            perm = np.concatenate([np.arange(16, 32), np.arange(0, 16)])
            cwl = cwl[::-1]
            sgw = sgw[:, ::-1, ::-1]
            sgb = sgb[:, ::-1]
        m = dict(shared)
        m["xT_own"] = np.ascontiguousarray(own.T, f32)
        m["xT_oth"] = np.ascontiguousarray(oth.T, f32)
        m["xT_ctx"] = np.ascontiguousarray(cx.T, f32)
        m["x_own"] = np.ascontiguousarray(own, f32)
        cc = np.stack([c[b], c_ctx], axis=1)
        m["cfm"] = np.ascontiguousarray(cc.reshape(16, 128, 2).transpose(1, 0, 2).reshape(128, 32), f32)
        m["wg"] = np.ascontiguousarray(wg_nat[:, perm], f32)
        m["gateb_bc"] = _bc(gate_b[0].reshape(32)[perm])
        m["convw_fm"] = np.ascontiguousarray(cwl.T.reshape(16, 128, 5).transpose(1, 0, 2).reshape(128, 80), f32)
        m["sgwT"] = np.ascontiguousarray(sgw.transpose(2, 0, 1).reshape(128, 1024), f32)
        m["sgb_row"] = np.ascontiguousarray(sgb.reshape(1, 1024), f32)
        if STOP_AFTER is None or STOP_AFTER >= 7:
            m["exp_w1"] = np.ascontiguousarray(exp_w1[0][4 * core:4 * core + 4], f32)
            m["exp_w2"] = np.ascontiguousarray(exp_w2[0][4 * core:4 * core + 4], f32)
        in_maps.append(m)
    return in_maps


def kernel(**inputs):
    inputs = {k: np.asarray(v) for k, v in inputs.items()}
    in_maps = prepare_inputs(**inputs)
    nc, _ = build_program()
    res = run_bass_kernel_spmd(nc, in_maps, core_ids=list(range(8)))
    out = np.empty((4, 2 * TOWN, D), np.float32)
    for core in range(8):
        b, half = core // 2, core % 2
        o = np.asarray(res.results[core]["out"], np.float32)
        if half == 1:
            o = o[::-1]
        out[b, half * TOWN:(half + 1) * TOWN] = o
    return out
```
